# Optimizing a Trainium2 kernel written in Bass

```python
import math
import numpy as np
import jax
import jax.numpy as jnp
from jax import lax

D_MODEL = 4096
BATCH = 2
SEQ = 4096
DEPTH = 1

MIX_WIDTH = D_MODEL
GA_WIDTH = MIX_WIDTH // 2
GA_HEADS = 16
GA_HEAD_DIM = GA_WIDTH // GA_HEADS
CHUNK = 128
NSA_WIDTH = MIX_WIDTH - GA_WIDTH
HEAD_DIM = 128
N_HEADS = NSA_WIDTH // HEAD_DIM
N_KV = 4
HPG = N_HEADS // N_KV
KV_WIDTH = N_KV * HEAD_DIM
L_CMP = 32
D_CMP = 16
L_SEL = 64
N_SEL = 16
WINDOW = 512
Q_BLOCK = 64
N_BRANCH = 3
N_BUCKETS = 32
MAX_DISTANCE = 128
N_EXPERTS = 32
TOP_K = 4
D_FF = D_MODEL // 2
SWIGLU_LIMIT = 7.0
SWIGLU_ALPHA = 1.702
LN_EPS = 1e-5
DEEPNORM_ALPHA = (2 * DEPTH) ** 0.25
DEEPNORM_BETA = (8 * DEPTH) ** -0.25
NEG_INF = -1e30
IN_SIZES = (GA_WIDTH, GA_WIDTH, NSA_WIDTH, KV_WIDTH, KV_WIDTH, KV_WIDTH, KV_WIDTH, KV_WIDTH, KV_WIDTH, N_HEADS * N_BRANCH)
IN_WIDTH = sum(IN_SIZES)

kernel_name = 'hybrid_gmlp_nsa_moe_deepnorm'


def layer_norm(x, g, b):
    xf = x.astype(jnp.float32)
    mu = jnp.mean(xf, axis=-1, keepdims=True)
    var = jnp.mean(jnp.square(xf - mu), axis=-1, keepdims=True)
    y = (xf - mu) * lax.rsqrt(var + LN_EPS) * g.astype(jnp.float32) + b.astype(jnp.float32)
    return y.astype(x.dtype)


def t5_bucket(dist):
    max_exact = N_BUCKETS // 2
    dist = jnp.maximum(dist, 0)
    scaled = jnp.log(jnp.maximum(dist, max_exact).astype(jnp.float32) / max_exact) / math.log(MAX_DISTANCE / max_exact)
    large = jnp.minimum(max_exact + (scaled * (N_BUCKETS - max_exact)).astype(jnp.int32), N_BUCKETS - 1)
    return jnp.where(dist < max_exact, dist, large)


def masked_softmax(s, mask):
    s = jnp.where(mask, s.astype(jnp.float32), NEG_INF)
    return jnp.where(mask, jax.nn.softmax(s, axis=-1), 0.0)


def split_columns(h):
    offs = [int(o) for o in np.cumsum(IN_SIZES)[:-1]]
    return jnp.split(h, offs, axis=-1)


def spatial_gating(u, v, ln_g, ln_b, w_s, b_s):
    B, S, _ = u.shape
    v = layer_norm(v, ln_g, ln_b).reshape(B, S // CHUNK, CHUNK, GA_HEADS, GA_HEAD_DIM)
    w = w_s * jnp.tril(jnp.ones((CHUNK, CHUNK), w_s.dtype))
    mixed = jnp.einsum('hij,bcjhd->bcihd', w, v) + b_s.T[:, :, None]
    return u * mixed.reshape(B, S, GA_WIDTH)


def compress(blocks, pos, w1, w2):
    h = (blocks + pos).reshape(blocks.shape[0], blocks.shape[1], blocks.shape[2], L_CMP * HEAD_DIM)
    return jax.nn.gelu(h @ w1) @ w2


def nsa(q, k_c, v_c, k_s, v_s, k_w, v_w, gate_logits, rel_bias, pos_k, w1_k, w2_k, pos_v, w1_v, w2_v):
    B, S, _ = q.shape

    def heads_kv(t):
        return t.reshape(B, S, N_KV, HEAD_DIM).transpose(0, 2, 1, 3)

    n_cmp = (S - L_CMP) // D_CMP + 1
    cmp_idx = np.arange(n_cmp)[:, None] * D_CMP + np.arange(L_CMP)[None, :]
    kc = compress(heads_kv(k_c)[:, :, cmp_idx], pos_k, w1_k, w2_k)
    vc = compress(heads_kv(v_c)[:, :, cmp_idx], pos_v, w1_v, w2_v)
    cmp_end = jnp.asarray(np.arange(n_cmp) * D_CMP + L_CMP - 1, jnp.int32)
    n_blk = S // L_SEL
    n_top = min(N_SEL, n_blk)
    ks = heads_kv(k_s).reshape(B, N_KV, n_blk, L_SEL, HEAD_DIM)
    vs = heads_kv(v_s).reshape(B, N_KV, n_blk, L_SEL, HEAD_DIM)
    ratio = L_SEL // D_CMP
    n_sum = L_CMP // D_CMP
    pad = ratio * n_blk + n_sum - 1 - n_cmp
    kw = jnp.pad(heads_kv(k_w), ((0, 0), (0, 0), (WINDOW, 0), (0, 0)))
    vw = jnp.pad(heads_kv(v_w), ((0, 0), (0, 0), (WINDOW, 0), (0, 0)))

    n_qb = S // Q_BLOCK
    qh = (q * (HEAD_DIM ** -0.5)).reshape(B, n_qb, Q_BLOCK, N_KV, HPG, HEAD_DIM).transpose(1, 0, 3, 4, 2, 5)
    gates = jax.nn.sigmoid(gate_logits.astype(jnp.float32)).reshape(B, n_qb, Q_BLOCK, N_KV, HPG, N_BRANCH).transpose(1, 0, 3, 4, 2, 5)
    bias_h = rel_bias.reshape(N_BUCKETS, N_KV, HPG)
    bias_g = bias_h.transpose(1, 0, 2)
    b_idx = jnp.arange(B)[:, None, None, None]
    g_idx = jnp.arange(N_KV)[None, :, None, None]
    blk = jnp.arange(n_blk)

    def block_step(args):
        c, qb, gb = args
        qpos = c * Q_BLOCK + jnp.arange(Q_BLOCK)
        dist = qpos[:, None] - cmp_end[None, :]
        s = jnp.einsum('bgeqd,bgkd->bgeqk', qb, kc).astype(jnp.float32) + bias_h[t5_bucket(dist)].transpose(2, 3, 0, 1)
        p_cmp = masked_softmax(s, dist >= 0)
        o_cmp = jnp.einsum('bgeqk,bgkd->bgeqd', p_cmp.astype(vc.dtype), vc)
        p_pad = jnp.pad(p_cmp.sum(axis=2), ((0, 0), (0, 0), (0, 0), (0, pad)))
        p_slc = p_pad[..., 0:ratio * n_blk].reshape(B, N_KV, Q_BLOCK, n_blk, ratio).sum(-1)
        for n in range(1, n_sum):
            p_slc = p_slc + p_pad[..., n:n + ratio * n_blk].reshape(B, N_KV, Q_BLOCK, n_blk, ratio).sum(-1)
        q_blk = qpos // L_SEL
        causal = blk[None, :] * L_SEL <= qpos[:, None]
        forced = (blk[None, :] == 0) | (blk[None, :] == q_blk[:, None]) | (blk[None, :] == q_blk[:, None] - 1)
        p_slc = jnp.where(causal, p_slc, -jnp.inf)
        p_slc = jnp.where(causal & forced, jnp.inf, p_slc)
        _, top = lax.top_k(p_slc, n_top)
        k_sel = ks[b_idx, g_idx, top].reshape(B, N_KV, Q_BLOCK, n_top * L_SEL, HEAD_DIM)
        v_sel = vs[b_idx, g_idx, top].reshape(B, N_KV, Q_BLOCK, n_top * L_SEL, HEAD_DIM)
        kpos = (top[..., None] * L_SEL + jnp.arange(L_SEL)).reshape(B, N_KV, Q_BLOCK, n_top * L_SEL)
        dist = qpos[:, None] - kpos
        bias = jnp.moveaxis(bias_g[g_idx, t5_bucket(dist)], -1, 2)
        s = jnp.einsum('bgeqd,bgqkd->bgeqk', qb, k_sel).astype(jnp.float32) + bias
        p = masked_softmax(s, (dist >= 0)[:, :, None])
        o_slc = jnp.einsum('bgeqk,bgqkd->bgeqd', p.astype(v_sel.dtype), v_sel)
        k_win = lax.dynamic_slice_in_dim(kw, c * Q_BLOCK, WINDOW + Q_BLOCK, axis=2)
        v_win = lax.dynamic_slice_in_dim(vw, c * Q_BLOCK, WINDOW + Q_BLOCK, axis=2)
        wpos = c * Q_BLOCK - WINDOW + jnp.arange(WINDOW + Q_BLOCK)
        dist = qpos[:, None] - wpos[None, :]
        mask = (dist >= 0) & (dist < WINDOW) & (wpos[None, :] >= 0)
        s = jnp.einsum('bgeqd,bgkd->bgeqk', qb, k_win).astype(jnp.float32) + bias_h[t5_bucket(dist)].transpose(2, 3, 0, 1)
        p = masked_softmax(s, mask)
        o_win = jnp.einsum('bgeqk,bgkd->bgeqd', p.astype(v_win.dtype), v_win)
        out = gb[..., 0:1] * o_cmp + gb[..., 1:2] * o_slc + gb[..., 2:3] * o_win
        return out.astype(qb.dtype)

    out = lax.map(block_step, (jnp.arange(n_qb), qh, gates))
    return out.transpose(1, 0, 4, 2, 3, 5).reshape(B, S, N_HEADS * HEAD_DIM)


def moe(x, w_router, b_router, w_gate_up, b_gate_up, w_down, b_down):
    B, S, D = x.shape
    xt = x.reshape(B * S, D)
    logits = (xt @ w_router + b_router).astype(jnp.float32)
    top_val, top_idx = lax.top_k(logits, TOP_K)
    top_w = jax.nn.softmax(top_val, axis=-1)
    gate = jnp.einsum('tk,tke->te', top_w, jax.nn.one_hot(top_idx, N_EXPERTS, dtype=jnp.float32))
    y = jnp.zeros((B * S, D), jnp.float32)
    for e in range(N_EXPERTS):
        h = xt @ w_gate_up[e] + b_gate_up[e]
        glu = jnp.minimum(h[:, 0::2], SWIGLU_LIMIT)
        lin = jnp.clip(h[:, 1::2], -SWIGLU_LIMIT, SWIGLU_LIMIT)
        act = glu * jax.nn.sigmoid(SWIGLU_ALPHA * glu) * (lin + 1.0)
        y = y + gate[:, e:e + 1] * (act @ w_down[e] + b_down[e]).astype(jnp.float32)
    return y.astype(x.dtype).reshape(B, S, D)


def setup_inputs(seed: int = 0) -> dict:
    key = jax.random.key(seed)
    ks = jax.random.split(key, 24)
    f32 = jnp.float32

    def nrm(k, shape, s):
        return jax.random.normal(k, shape, f32) * s

    beta = DEEPNORM_BETA
    col_scale = np.concatenate([np.full(n, s, np.float32) for n, s in zip(IN_SIZES, (1.0, 1.0, 1.0, 1.0, beta, 1.0, beta, 1.0, beta, 1.0))])
    return {
        'x': nrm(ks[0], (BATCH, SEQ, D_MODEL), 1.0),
        'w_in': nrm(ks[1], (DEPTH, D_MODEL, IN_WIDTH), D_MODEL ** -0.5) * jnp.asarray(col_scale),
        'ga_ln_g': 1.0 + nrm(ks[2], (DEPTH, GA_WIDTH), 0.02),
        'ga_ln_b': nrm(ks[3], (DEPTH, GA_WIDTH), 0.02),
        'ga_w_s': nrm(ks[4], (DEPTH, GA_HEADS, CHUNK, CHUNK), CHUNK ** -0.5),
        'ga_b_s': 1.0 + nrm(ks[5], (DEPTH, GA_HEADS, CHUNK), 0.02),
        'cmp_pos_k': nrm(ks[6], (DEPTH, L_CMP, HEAD_DIM), 0.1),
        'cmp_w1_k': nrm(ks[7], (DEPTH, L_CMP * HEAD_DIM, HEAD_DIM), (L_CMP * HEAD_DIM) ** -0.5),
        'cmp_w2_k': nrm(ks[8], (DEPTH, HEAD_DIM, HEAD_DIM), HEAD_DIM ** -0.5),
        'cmp_pos_v': nrm(ks[9], (DEPTH, L_CMP, HEAD_DIM), 0.1),
        'cmp_w1_v': nrm(ks[10], (DEPTH, L_CMP * HEAD_DIM, HEAD_DIM), (L_CMP * HEAD_DIM) ** -0.5),
        'cmp_w2_v': nrm(ks[11], (DEPTH, HEAD_DIM, HEAD_DIM), HEAD_DIM ** -0.5),
        'rel_bias': nrm(ks[12], (N_BUCKETS, N_HEADS), 0.5),
        'w_out': nrm(ks[13], (DEPTH, MIX_WIDTH, D_MODEL), MIX_WIDTH ** -0.5 * beta),
        'ln1_g': 1.0 + nrm(ks[14], (DEPTH, D_MODEL), 0.02),
        'ln1_b': nrm(ks[15], (DEPTH, D_MODEL), 0.02),
        'w_router': nrm(ks[16], (DEPTH, D_MODEL, N_EXPERTS), D_MODEL ** -0.5),
        'b_router': nrm(ks[17], (DEPTH, N_EXPERTS), 0.01),
        'w_gate_up': nrm(ks[18], (DEPTH, N_EXPERTS, D_MODEL, 2 * D_FF), D_MODEL ** -0.5 * beta),
        'b_gate_up': nrm(ks[19], (DEPTH, N_EXPERTS, 2 * D_FF), 0.01),
        'w_down': nrm(ks[20], (DEPTH, N_EXPERTS, D_FF, D_MODEL), D_FF ** -0.5 * beta),
        'b_down': nrm(ks[21], (DEPTH, N_EXPERTS, D_MODEL), 0.01),
        'ln2_g': 1.0 + nrm(ks[22], (DEPTH, D_MODEL), 0.02),
        'ln2_b': nrm(ks[23], (DEPTH, D_MODEL), 0.02),
    }


def reference(x, w_in, ga_ln_g, ga_ln_b, ga_w_s, ga_b_s, cmp_pos_k, cmp_w1_k, cmp_w2_k, cmp_pos_v, cmp_w1_v, cmp_w2_v, rel_bias, w_out, ln1_g, ln1_b, w_router, b_router, w_gate_up, b_gate_up, w_down, b_down, ln2_g, ln2_b):
    for layer in range(DEPTH):
        h = jnp.einsum('bsd,df->bsf', x, w_in[layer])
        u_a, v_a, q, k_c, v_c, k_s, v_s, k_w, v_w, gate_logits = split_columns(h)
        y_a = spatial_gating(jax.nn.gelu(u_a), jax.nn.gelu(v_a), ga_ln_g[layer], ga_ln_b[layer], ga_w_s[layer], ga_b_s[layer])
        y_b = nsa(q, k_c, v_c, k_s, v_s, k_w, v_w, gate_logits, rel_bias,
                  cmp_pos_k[layer], cmp_w1_k[layer], cmp_w2_k[layer], cmp_pos_v[layer], cmp_w1_v[layer], cmp_w2_v[layer])
        mix = jnp.einsum('bsf,fd->bsd', jnp.concatenate([y_a, y_b.astype(y_a.dtype)], axis=-1), w_out[layer])
        x = layer_norm(DEEPNORM_ALPHA * x + mix, ln1_g[layer], ln1_b[layer])
        ffn = moe(x, w_router[layer], b_router[layer], w_gate_up[layer], b_gate_up[layer], w_down[layer], b_down[layer])
        x = layer_norm(DEEPNORM_ALPHA * x + ffn, ln2_g[layer], ln2_b[layer])
    return x
```

```python
import math
from contextlib import ExitStack

import numpy as np
import ml_dtypes

import concourse.bass as bass
import concourse.mybir as mybir
from concourse.bass_utils import run_bass_kernel_spmd

F32 = mybir.dt.float32
BF16 = mybir.dt.bfloat16
AF = mybir.ActivationFunctionType
ALU = mybir.AluOpType
AX = mybir.AxisListType
P = 128
NEG = -30000.0
ALPHA = 2.0 ** 0.25
LN_EPS = 1e-5
N_CORES = 8


class Cfg:
    def __init__(self, D=4096, S=4096, NE=32, DFF=2048):
        self.D, self.S, self.NE, self.DFF = D, S, NE, DFF
        self.B = 2
        self.T = self.B * S
        self.TOK = self.T // N_CORES
        self.KC = D // P
        self.EPC = NE // N_CORES
        self.NQT = S // P
        self.NCMP = (S - 32) // 16 + 1
        self.NBLK = S // 64
        self.FC = DFF // P
        self.NOH = P * 656


FULL = Cfg()


EXCL_PSUM = True


class Buf:
    __slots__ = ("w", "r", "excl")

    def __init__(self, excl=False):
        self.w = None
        self.r = {}
        self.excl = excl and EXCL_PSUM


class Sched:
    def __init__(self, nc, n_dma_sems=28):
        self.nc = nc
        self.eng = {"pe": nc.tensor, "act": nc.scalar, "dve": nc.vector, "pool": nc.gpsimd, "sp": nc.sync}
        self.sem, self.cnt = {}, {}
        self.seen = {k: {} for k in self.eng}
        for k in ("pe", "act", "dve", "pool"):
            self.sem[k] = nc.alloc_semaphore("s_" + k)
            self.cnt[k] = 0
        self.dsem = [nc.alloc_semaphore("d%d" % i) for i in range(n_dma_sems)]
        self.dval = [0] * n_dma_sems
        self.dnext = 0
        self.n_rot = n_dma_sems
        self.dnext_p = 0
        self.cc_tickets = []

    def wait(self, e, t):
        sem, val = t
        if self.seen[e].get(sem.num, 0) >= val:
            return
        self.eng[e].wait_ge(sem, val)
        self.seen[e][sem.num] = val

    def _deps(self, e, reads, writes):
        deps = []
        for b in reads:
            if b.w is not None:
                deps.append(b.w)
            if b.excl:
                deps.extend(b.r.values())
        for b in writes:
            if b.w is not None:
                deps.append(b.w)
            deps.extend(b.r.values())
        for t in deps:
            if e == "pe" and t[0].num == self.sem["pe"].num:
                continue
            self.wait(e, t)

    @staticmethod
    def _mark(t, reads, writes):
        for b in reads:
            if b.excl:
                b.w = t
                b.r = {}
            else:
                b.r[t[0].num] = t
        for b in writes:
            b.w = t
            b.r = {}

    def op(self, e, fn, reads=(), writes=()):
        return self.group(e, (fn,), reads, writes)

    def group(self, e, fns, reads=(), writes=()):
        self._deps(e, reads, writes)
        ins = None
        for fn in fns:
            ins = fn()
        ins.then_inc(self.sem[e], 1)
        self.cnt[e] += 1
        t = (self.sem[e], self.cnt[e])
        self._mark(t, reads, writes)
        return t

    FRESH_POOL_SEMS = 0

    def dma(self, q, out, in_, reads=(), writes=()):
        if q == "pool" and Sched.FRESH_POOL_SEMS > 0:
            Sched.FRESH_POOL_SEMS -= 1
            self.dsem.append(self.nc.alloc_semaphore("dp%d" % len(self.dsem)))
            self.dval.append(0)
            i = len(self.dsem) - 1
        else:
            i = self.dnext
            self.dnext = (self.dnext + 1) % self.n_rot
        sem = self.dsem[i]
        if self.dval[i] > 0:
            self.wait(q, (sem, self.dval[i]))
        self._deps(q, reads, writes)
        ins = self.eng[q].dma_start(out=out, in_=in_)
        ins.then_inc(sem, 16)
        self.dval[i] += 16
        t = (sem, self.dval[i])
        self._mark(t, reads, writes)
        return t

    def cc(self, kind, op, groups, in_ap, out_ap, reads=(), writes=()):
        sem = self.nc.alloc_semaphore("cc%d" % len(self.cc_tickets))
        self._deps("pool", reads, writes)
        ins = self.nc.gpsimd.collective_compute(kind, op, replica_groups=groups, ins=[in_ap.opt()], outs=[out_ap.opt()])
        ins.then_inc(sem)
        t = (sem, 1)
        self.cc_tickets.append(t)
        self._mark(t, reads, writes)
        return t

    def all_tickets(self):
        ts = [(self.sem[k], self.cnt[k]) for k in self.sem if self.cnt[k] > 0]
        ts += [(s, v) for s, v in zip(self.dsem, self.dval) if v > 0]
        ts += self.cc_tickets
        return ts

    def barrier(self):
        ts = self.all_tickets()
        for e in self.eng:
            for t in ts:
                self.wait(e, t)


class Ring:
    def __init__(self, es, nc, name, shape, dtype, n):
        self.t = [es.enter_context(nc.sbuf_tensor("%s%d" % (name, i), shape, dtype)) for i in range(n)]
        self.b = [Buf() for _ in range(n)]
        self.i = 0

    def next(self):
        k = self.i
        self.i = (self.i + 1) % len(self.t)
        return self.t[k], self.b[k]


def ceil_div(a, b):
    return (a + b - 1) // b


CC_MAX_BYTES = 32 << 20


def split_distinct(n, m):
    sizes, rem, cur = [], n, m
    while rem > 0:
        take = min(cur, rem)
        assert take > 0
        sizes.append(take)
        rem -= take
        cur = take - 1
    return sizes


def pieces_over(sizes, c0, c1):
    out, off = [], 0
    for j, w in enumerate(sizes):
        a, b = max(c0, off), min(c1, off + w)
        if a < b:
            out.append((j, a - off, b - off, a, b))
        off += w
    return out


def t5_bucket_np(dist):
    dist = np.maximum(dist, 0)
    scaled = np.log(np.maximum(dist, 16).astype(np.float32) / np.float32(16)) / np.float32(math.log(128 / 16))
    large = np.minimum(16 + (scaled * np.float32(16)).astype(np.int32), 31)
    return np.where(dist < 16, dist, large)


def static_tables(cfg):
    ql = np.arange(P)[:, None]
    kl = np.arange(640)[None, :]
    dw = ql + 512 - kl
    mi = np.arange(16)[None, :]
    dc = ql - 31 - 16 * (mi - 9)
    oh = np.zeros((65, cfg.NOH), np.float32)
    for dist, valid, off, width in ((dw, (dw >= 0) & (dw < 512), 0, 640), (dc, dc >= 0, P * 640, 16)):
        bk = t5_bucket_np(dist)
        idx = (off + ql * width + np.arange(width)[None, :])
        for q in range(P):
            for k in range(width):
                col = idx[q, k]
                if valid[q, k]:
                    oh[bk[q, k], col] = 1.0
                    oh[32 + bk[q, k], col] = 1.0
                else:
                    oh[64, col] = 1.0
    sm = np.zeros((P, cfg.NQT, cfg.NBLK), np.float32)
    blk = np.arange(cfg.NBLK)
    for t in range(cfg.NQT):
        qpos = t * P + np.arange(P)
        qb = qpos // 64
        causal = blk[None, :] * 64 <= qpos[:, None]
        m = np.where(causal, 0.0, -1e9).astype(np.float32)
        m = np.where(causal & (blk[None, :] == qb[:, None] - 1), 192.0, m)
        m = np.where(causal & (blk[None, :] == qb[:, None]), 128.0, m)
        m = np.where(causal & (blk[None, :] == 0), 64.0, m)
        sm[:, t, :] = m
    tril = (np.arange(P)[:, None] <= np.arange(P)[None, :]).astype(np.float32)
    return {
        "onehot": oh.astype(ml_dtypes.bfloat16),
        "selmask": sm.reshape(P, cfg.NQT * cfg.NBLK),
        "trilT": tril,
        "ident": np.eye(P, dtype=np.float32),
    }


def host_inputs(cfg, inp):
    D, S, TOK, EPC, NE, FC = cfg.D, cfg.S, cfg.TOK, cfg.EPC, cfg.NE, cfg.FC
    f = lambda a: np.ascontiguousarray(np.asarray(a, dtype=np.float32))
    x = f(inp["x"])
    x2 = x.reshape(cfg.T, D)
    w_in = f(inp["w_in"])[0]
    w_out = f(inp["w_out"])[0]
    st = static_tables(cfg)
    rel = f(inp["rel_bias"])
    wgu = np.asarray(inp["w_gate_up"])[0]
    wd = np.asarray(inp["w_down"])[0]
    bgu = f(inp["b_gate_up"])[0]
    bd = f(inp["b_down"])[0]
    maps = []
    for c in range(N_CORES):
        b, g = c // 4, c % 4
        fm_cols = np.concatenate([np.arange(4096 + g * 512, 4096 + (g + 1) * 512)] +
                                 [np.arange(o + g * 128, o + (g + 1) * 128) for o in (6144, 6656, 7168, 8192)])
        tm_cols = np.concatenate([np.arange(7680 + g * 128, 7680 + (g + 1) * 128),
                                  np.arange(8704 + g * 128, 8704 + (g + 1) * 128),
                                  np.arange(9216 + 12 * g, 9216 + 12 * (g + 1))])
        own = slice(c * TOK, (c + 1) * TOK)
        ex = slice(c * EPC, (c + 1) * EPC)
        selB = np.zeros((EPC, NE, P), np.float32)
        bdsel = np.zeros((NE, D), np.float32)
        for i in range(EPC):
            selB[i, c * EPC + i, :] = 1.0
            bdsel[c * EPC + i] = bd[c * EPC + i]
        bgu_l = bgu[ex].reshape(EPC, FC, P, 2).transpose(2, 0, 3, 1)
        m = {
            "xT": f(x[b].T),
            "xown": f(x2[own]),
            "xTown": f(x2[own].T),
            "w_fm": f(w_in[:, fm_cols]),
            "w_tm": f(w_in[:, tm_cols]),
            "w_ga": f(w_in[:, 0:4096]),
            "ga_ln_g": f(inp["ga_ln_g"]).reshape(1, 2048),
            "ga_ln_b": f(inp["ga_ln_b"]).reshape(1, 2048),
            "ga_wT": f(f(inp["ga_w_s"])[0].transpose(2, 0, 1)).reshape(P, 16 * P),
            "ga_bs": f(inp["ga_b_s"]).reshape(1, 2048),
            "posT_k": f(f(inp["cmp_pos_k"])[0].T),
            "posT_v": f(f(inp["cmp_pos_v"])[0].T),
            "w1_k": f(f(inp["cmp_w1_k"])[0].reshape(32, P, P).transpose(1, 0, 2)).reshape(P, 32 * P),
            "w1_v": f(f(inp["cmp_w1_v"])[0].reshape(32, P, P).transpose(1, 0, 2)).reshape(P, 32 * P),
            "w2_k": f(f(inp["cmp_w2_k"])[0]),
            "w2_v": f(f(inp["cmp_w2_v"])[0]),
            "tbl": f(np.concatenate([rel[:, 4 * g:4 * g + 4]] * 2, 0)),
            "tbl31": f(np.repeat(rel[31:32, 4 * g:4 * g + 4], 64, 0)),
            "w_out_a": f(w_out[0:2048]),
            "w_out_b": f(w_out[2048 + g * 512:2048 + (g + 1) * 512]),
            "ln1_g": f(inp["ln1_g"]).reshape(1, D), "ln1_b": f(inp["ln1_b"]).reshape(1, D),
            "ln2_g": f(inp["ln2_g"]).reshape(1, D), "ln2_b": f(inp["ln2_b"]).reshape(1, D),
            "w_router": f(f(inp["w_router"])[0]),
            "b_router": f(inp["b_router"]).reshape(1, NE),
            "selB": selB.reshape(EPC * NE, P),
            "bdsel": bdsel,
            "wgu": f(wgu[ex]).reshape(EPC * D, 2 * cfg.DFF),
            "bgu": f(bgu_l).reshape(P, EPC * 2 * FC),
            "wd": f(wd[ex]).reshape(EPC * cfg.DFF, D),
        }
        m.update(st)
        maps.append(m)
    return maps


PHASES = ['0', 'A', 'A2', 'GA', 'GB', 'C', 'D1', 'D2', 'D3', 'E', 'F', 'G']


def build(cfg, upto='G'):
    last = PHASES.index(upto)
    on = lambda ph: PHASES.index(ph) <= last
    D, S, T, TOK, KC, EPC, NE, FC, DFF = cfg.D, cfg.S, cfg.T, cfg.TOK, cfg.KC, cfg.EPC, cfg.NE, cfg.FC, cfg.DFF
    NQT, NCMP, NBLK, NOH = cfg.NQT, cfg.NCMP, cfg.NBLK, cfg.NOH
    NCC = ceil_div(NCMP, P)
    nc = bass.Bass("TRN2", target_bir_lowering=False)
    S_ = Sched(nc)

    def din(name, shape, dt=F32):
        return nc.dram_tensor(name, list(shape), dt, kind="ExternalInput").ap()

    def dscr(name, shape, dt=F32, shared=False):
        return nc.dram_tensor(name, list(shape), dt, addr_space=("Shared" if shared else "Local")).ap()

    xT = din("xT", [D, S]); xown = din("xown", [TOK, D]); xTown = din("xTown", [D, TOK])
    w_fm = din("w_fm", [D, 1024]); w_tm = din("w_tm", [D, 268]); w_ga = din("w_ga", [D, 4096])
    ga_ln_g = din("ga_ln_g", [1, 2048]); ga_ln_b = din("ga_ln_b", [1, 2048])
    ga_wT = din("ga_wT", [P, 16 * P]); ga_bs = din("ga_bs", [1, 2048])
    posT = {"k": din("posT_k", [P, 32]), "v": din("posT_v", [P, 32])}
    w1 = {"k": din("w1_k", [P, 32 * P]), "v": din("w1_v", [P, 32 * P])}
    w2 = {"k": din("w2_k", [P, P]), "v": din("w2_v", [P, P])}
    tbl = din("tbl", [64, 4]); tbl31 = din("tbl31", [64, 4])
    onehot = din("onehot", [65, NOH], BF16)
    selmask = din("selmask", [P, NQT * NBLK]); trilT = din("trilT", [P, P]); ident = din("ident", [P, P])
    w_out_a = din("w_out_a", [2048, D]); w_out_b = din("w_out_b", [512, D])
    ln1_g = din("ln1_g", [1, D]); ln1_b = din("ln1_b", [1, D]); ln2_g = din("ln2_g", [1, D]); ln2_b = din("ln2_b", [1, D])
    w_router = din("w_router", [D, NE]); b_router = din("b_router", [1, NE])
    selB = din("selB", [EPC * NE, P]); bdsel = din("bdsel", [NE, D])
    wgu = din("wgu", [EPC * D, 2 * DFF]); bgu = din("bgu", [P, EPC * 2 * FC]); wd = din("wd", [EPC * DFF, D])
    out = nc.dram_tensor("out", [TOK, D], F32, kind="ExternalOutput").ap()

    bias_d = dscr("bias_d", [4, NOH])
    qT_d = dscr("qT_d", [512, S], BF16)
    kT_d = {k: dscr("kT_" + k, [P, S], BF16) for k in ("kc", "vc", "ks", "kw")}
    vs_d = dscr("vs_d", [S, P], BF16); vw_d = dscr("vw_d", [S, P], BF16); gts_d = dscr("gts_d", [S, 12])
    guT_d = dscr("guT_d", [2048, TOK], BF16); gv_d = dscr("gv_d", [TOK, 2048]); yaT_d = dscr("yaT_d", [2048, TOK], BF16)
    NCB = D // 512
    W1 = [w * P for w in split_distinct(D // P, max(1, CC_MAX_BYTES // (S * 4 * P)))]
    W2 = [w * P for w in split_distinct(D // P, max(1, CC_MAX_BYTES // (T * 4 * P)))]
    KA = split_distinct(KC, max(1, CC_MAX_BYTES // (N_CORES * P * TOK * 2)))
    KA0 = [sum(KA[:j]) for j in range(len(KA))]
    rs1_in = [dscr("rs1_in_" + "abcdefghij"[k], [S, w]) for k, w in enumerate(W1)]; rs1_out = [dscr("rs1_out_" + "abcdefghij"[k], [TOK, w]) for k, w in enumerate(W1)]
    z1_d = dscr("z1_d", [TOK, D]); x1_d = dscr("x1_d", [TOK, D])
    agx_in = [dscr("agx_in_" + "abcdefghij"[j], [nk * P, TOK], BF16) for j, nk in enumerate(KA)]
    agx = [dscr("agx_" + "abcdefghij"[j], [N_CORES * nk * P, TOK], BF16, shared=True) for j, nk in enumerate(KA)]
    agg_in = dscr("agg_in", [NE, TOK]); agg = dscr("agg", [N_CORES * NE, TOK], shared=True)
    act_d = dscr("act_d", [EPC * DFF, T], BF16)
    part_d = [dscr("part_d_" + "abcdefghij"[k], [T, w]) for k, w in enumerate(W2)]; ffn_d = [dscr("ffn_d_" + "abcdefghij"[k], [TOK, w]) for k, w in enumerate(W2)]
    D_ = {n: Buf() for n in ("bias", "qT", "kc", "vc", "ks", "kw", "vs", "vw", "gts", "guT", "gv", "yaT", "rs1_in", "rs1_out",
                             "z1", "x1", "agx_in", "agx", "agg_in", "agg", "act", "part", "ffn")}
    for k in range(max(len(W1), len(W2), len(KA))):
        for n_ in ("rs1_in", "rs1_out", "part", "ffn", "agx_in", "agx"):
            D_["%s%d" % (n_, k)] = Buf()

    def dbg_return():
        scr = {"bias": bias_d, "qT": qT_d, "kc": kT_d["kc"], "vc": kT_d["vc"], "ks": kT_d["ks"], "kw": kT_d["kw"], "vs": vs_d, "vw": vw_d, "gts": gts_d,
               "guT": guT_d, "gv": gv_d, "yaT": yaT_d, "z1": z1_d, "x1": x1_d, "agg": agg, "act": act_d}
        for n_, ap_ in scr.items():
            if D_[n_].w is None:
                continue
            o_ = nc.dram_tensor("dbg_" + n_, list(ap_.shape), ap_.dtype, kind="ExternalOutput").ap()
            S_.dma("sp", o_, ap_, [D_[n_]], [Buf()])
        for n_, (ap_, shp_, dt_, b_) in extra.items():
            o_ = nc.dram_tensor("dbg_" + n_, shp_, dt_, kind="ExternalOutput").ap()
            S_.dma("sp", o_, ap_, [b_], [Buf()])
        S_.barrier()
        return nc

    extra = {}

    top = ExitStack()
    sb = lambda es, name, shape, dt: es.enter_context(nc.sbuf_tensor(name, list(shape), dt))

    ident_f = sb(top, "ident_f", [P, P], F32); ident_b = sb(top, "ident_b", [P, P], BF16)
    Bwin = sb(top, "Bwin", [P, 4, 640], F32); Bcmp = sb(top, "Bcmp", [P, 4, 16], F32)
    kcT_sb = sb(top, "kcT_sb", [P, NCC * P], BF16); vc_sb = sb(top, "vc_sb", [P, NCC, P], BF16)
    ones_f = sb(top, "ones_f", [1, P], F32)
    b_ident, b_B, b_kc, b_vc, b_ones = Buf(), Buf(), Buf(), Buf(), Buf()
    S_.dma("sp", ident_f[:], ident, writes=[b_ident])
    S_.op("dve", lambda: nc.vector.tensor_copy(out=ident_b[:], in_=ident_f[:]), reads=[b_ident], writes=[b_ident])
    S_.op("dve", lambda: nc.vector.memset(ones_f[:], 1.0), writes=[b_ones])

    ps_ctr = {"i": 0}

    def psum_banks(es, n=8):
        ps_ctr["i"] += 1
        ps = es.enter_context(nc.psum_tensor("ps%d" % ps_ctr["i"], [P, n * 512], F32))
        return ps, [Buf(True) for _ in range(n)]

    cp_state = {"i": 0}

    def evac(out_ap, in_ap, reads, writes, scale=None, func=None, eng=None):
        if func is not None or scale is not None or eng == "act":
            fn = func if func is not None else AF.Copy
            kw = {} if scale is None else {"scale": float(scale)}
            return S_.op("act", lambda: nc.scalar.activation(out=out_ap, in_=in_ap, func=fn, **kw), reads, writes)
        if eng == "dve":
            return S_.op("dve", lambda: nc.vector.tensor_copy(out=out_ap, in_=in_ap), reads, writes)
        cp_state["i"] ^= 1
        if cp_state["i"]:
            return S_.op("act", lambda: nc.scalar.activation(out=out_ap, in_=in_ap, func=AF.Copy), reads, writes)
        return S_.op("dve", lambda: nc.vector.tensor_copy(out=out_ap, in_=in_ap), reads, writes)

    def layer_norm(es_tag, x_t, b_x, n, g_t, bta_t, b_gb, out_ap, b_out, small):
        st, b_st = small.next()
        S_.op("dve", lambda: nc.vector.memset(st[:], 0.0), writes=[b_st])
        junk, b_j = es_tag.next()
        S_.op("act", lambda: nc.scalar.activation(out=junk[:, 0:n], in_=x_t, func=AF.Copy, accum_out=st[:, 0:1]), [b_x], [b_j, b_st])
        S_.op("dve", lambda: nc.vector.tensor_scalar(out=st[:, 1:2], in0=st[:, 0:1], scalar1=-1.0 / n, scalar2=None, op0=ALU.mult), [b_st], [b_st])
        S_.op("act", lambda: nc.scalar.activation(out=junk[:, 0:n], in_=x_t, func=AF.Square, bias=st[:, 1:2], accum_out=st[:, 2:3]), [b_x, b_st], [b_j, b_st])
        S_.op("dve", lambda: nc.vector.tensor_scalar(out=st[:, 3:4], in0=st[:, 2:3], scalar1=1.0 / n, scalar2=LN_EPS, op0=ALU.mult, op1=ALU.add), [b_st], [b_st])
        S_.op("act", lambda: nc.scalar.activation(out=st[:, 4:5], in_=st[:, 3:4], func=AF.Sqrt), [b_st], [b_st])
        S_.op("dve", lambda: nc.vector.reciprocal(out=st[:, 5:6], in_=st[:, 4:5]), [b_st], [b_st])
        S_.op("dve", lambda: nc.vector.tensor_scalar(out=x_t, in0=x_t, scalar1=st[:, 1:2], scalar2=st[:, 5:6], op0=ALU.add, op1=ALU.mult), [b_x, b_st], [b_x])
        S_.op("dve", lambda: nc.vector.tensor_tensor(out=x_t, in0=x_t, in1=g_t, op=ALU.mult), [b_x, b_gb], [b_x])
        S_.op("dve", lambda: nc.vector.tensor_tensor(out=out_ap, in0=x_t, in1=bta_t, op=ALU.add), [b_x, b_gb], [b_out])

    with ExitStack() as es:
        ps, PB = psum_banks(es)
        t64 = sb(es, "t64", [64, 4], F32); t31 = sb(es, "t31", [64, 4], F32)
        hi = sb(es, "hi", [64, 4], BF16); hif = sb(es, "hif", [64, 4], F32); lhs65 = sb(es, "lhs65", [65, 4], BF16)
        b_t, b_l = Buf(), Buf()
        S_.dma("sp", t64[:], tbl, writes=[b_t]); S_.dma("sp", t31[:], tbl31, writes=[b_t])
        S_.op("dve", lambda: nc.vector.tensor_tensor(out=t64[:], in0=t64[:], in1=t31[:], op=ALU.subtract), [b_t], [b_t])
        S_.op("dve", lambda: nc.vector.tensor_copy(out=hi[:], in_=t64[:]), [b_t], [b_l])
        S_.op("dve", lambda: nc.vector.tensor_copy(out=hif[:], in_=hi[:]), [b_l], [b_l])
        S_.op("dve", lambda: nc.vector.tensor_tensor(out=t31[:], in0=t64[:], in1=hif[:], op=ALU.subtract), [b_t, b_l], [b_t])
        S_.op("dve", lambda: nc.vector.tensor_copy(out=lhs65[0:32, :], in_=hi[0:32, :]), [b_l], [b_l])
        S_.op("dve", lambda: nc.vector.tensor_copy(out=lhs65[32:64, :], in_=t31[32:64, :]), [b_t], [b_l])
        S_.op("dve", lambda: nc.vector.memset(lhs65[64:65, :], NEG), (), [b_l])
        ohr = Ring(es, nc, "oh", [65, 2048], BF16, 3)
        str_ = Ring(es, nc, "bst", [4, 2048], F32, 2)
        nchunk = NOH // 2048
        for ci in range(nchunk):
            oht, b_oh = ohr.next()
            S_.dma("sp", oht[:], onehot[:, ci * 2048:(ci + 1) * 2048], writes=[b_oh])
            bk = (ci % 2) * 4
            S_.group("pe", [(lambda j=j: nc.tensor.matmul(ps[0:4, (bk + j) * 512:(bk + j + 1) * 512], lhsT=lhs65[:], rhs=oht[:, j * 512:(j + 1) * 512], start=True, stop=True)) for j in range(4)],
                     [b_l, b_oh], PB[bk:bk + 4])
            stg, b_s = str_.next()
            evac(stg[:], ps[0:4, bk * 512:(bk + 4) * 512], PB[bk:bk + 4], [b_s])
            S_.dma("sp", bias_d[:, ci * 2048:(ci + 1) * 2048], stg[:], [b_s], [D_["bias"]])
        for h in range(4):
            S_.dma("sp", Bwin[:, h, :], bias_d[h:h + 1, 0:P * 640].rearrange("o (q k) -> (o q) k", k=640), [D_["bias"]], [b_B])
            S_.dma("sp", Bcmp[:, h, :], bias_d[h:h + 1, P * 640:P * 656].rearrange("o (q k) -> (o q) k", k=16), [D_["bias"]], [b_B])
        S_.barrier()

    if upto == '0':
        return dbg_return()

    QSCALE = 128.0 ** -0.5
    with ExitStack() as es:
        ps, PB = psum_banks(es)
        wfm = sb(es, "wfm", [P, KC, 1024], BF16); wtm = sb(es, "wtm", [P, KC, 268], BF16)
        b_wfm, b_wtm = Buf(), Buf()
        wv = w_fm.rearrange("(kc p) n -> p kc n", p=P); wtv = w_tm.rearrange("(kc p) n -> p kc n", p=P)
        KG = min(8, KC)
        for k0 in range(0, KC, KG):
            S_.dma("pool", wfm[:, k0:k0 + KG, :], wv[:, k0:k0 + KG, :], writes=[b_wfm])
        for k0 in range(0, KC, KG):
            S_.dma("pool", wtm[:, k0:k0 + KG, :], wtv[:, k0:k0 + KG, :], writes=[b_wtm])
        xr = Ring(es, nc, "xTb", [P, KC, 512], BF16, 2)
        st_fm = Ring(es, nc, "stfm", [P, 512], BF16, 4)
        st_v = Ring(es, nc, "stv", [P, 256], BF16, 3)
        st_g = Ring(es, nc, "stg", [P, 12], F32, 3)
        xTv = xT.rearrange("(kc p) s -> p kc s", p=P)
        NTB = S // 512
        fm_dst = [(qT_d[e * P:(e + 1) * P, :], "qT") for e in range(4)] + [(kT_d[k], k) for k in ("kc", "vc", "ks", "kw")]
        bank = 0

        def load_x(tb):
            xt, b_x = xr.next()
            for k0 in range(0, KC, KG):
                S_.dma("pool", xt[:, k0:k0 + KG, :], xTv[:, k0:k0 + KG, tb * 512:(tb + 1) * 512], writes=[b_x])
            return xt, b_x

        nxt = load_x(0)
        for tb in range(NTB):
            xt, b_x = nxt
            if tb + 1 < NTB:
                nxt = load_x(tb + 1)
            for cc in range(8):
                bk = bank; bank = (bank + 1) % 8
                S_.group("pe", [(lambda kc=kc: nc.tensor.matmul(ps[:, bk * 512:(bk + 1) * 512], lhsT=wfm[:, kc, cc * P:(cc + 1) * P], rhs=xt[:, kc, :], start=(kc == 0), stop=(kc == KC - 1))) for kc in range(KC)],
                         [b_wfm, b_x], [PB[bk]])
                stg, b_s = st_fm.next()
                evac(stg[:], ps[:, bk * 512:(bk + 1) * 512], [PB[bk]], [b_s], scale=(QSCALE if cc < 4 else None))
                dst, nm = fm_dst[cc]
                S_.dma("sp", dst[:, tb * 512:(tb + 1) * 512], stg[:], [b_s], [D_[nm]])
            for tt in range(4):
                bk = bank; bank = (bank + 1) % 8
                S_.group("pe", [(lambda kc=kc: nc.tensor.matmul(ps[:, bk * 512:bk * 512 + 268], lhsT=xt[:, kc, tt * P:(tt + 1) * P], rhs=wtm[:, kc, :], start=(kc == 0), stop=(kc == KC - 1))) for kc in range(KC)],
                         [b_wtm, b_x], [PB[bk]])
                sv, b_sv = st_v.next(); sg, b_sg = st_g.next()
                evac(sv[:], ps[:, bk * 512:bk * 512 + 256], [PB[bk]], [b_sv])
                evac(sg[:], ps[:, bk * 512 + 256:bk * 512 + 268], [PB[bk]], [b_sg], func=AF.Sigmoid)
                r0 = tb * 512 + tt * P
                S_.dma("sp", vs_d[r0:r0 + P, :], sv[:, 0:P], [b_sv], [D_["vs"]])
                S_.dma("sp", vw_d[r0:r0 + P, :], sv[:, P:2 * P], [b_sv], [D_["vw"]])
                S_.dma("sp", gts_d[r0:r0 + P, :], sg[:], [b_sg], [D_["gts"]])
        S_.barrier()

    if upto == 'A':
        return dbg_return()

    with ExitStack() as es:
        ps, PB = psum_banks(es)
        for kind in ("k", "v"):
            kf = sb(es, "kf" + kind, [P, S], BF16); w1s = sb(es, "w1" + kind, [P, 32, P], BF16); w2s = sb(es, "w2" + kind, [P, P], BF16)
            pT_ = sb(es, "pT" + kind, [P, 32], BF16); hb = sb(es, "hb" + kind, [P, 1], F32); gk = sb(es, "gk" + kind, [P, NCC * P], BF16)
            b_in, b_hb, b_gk = Buf(), Buf(), Buf()
            S_.dma("sp", kf[:], kT_d["kc" if kind == "k" else "vc"], [D_["kc" if kind == "k" else "vc"]], [b_in])
            S_.dma("pool", w1s[:], w1[kind].rearrange("p (l f) -> p l f", f=P), writes=[b_in])
            S_.dma("pool", w2s[:], w2[kind], writes=[b_in])
            S_.dma("pool", pT_[:], posT[kind], writes=[b_in])
            b0, b1, b2 = (0, 1, 2) if kind == "k" else (3, 4, 5)
            S_.group("pe", [(lambda l=l: nc.tensor.matmul(ps[:, b0 * 512:b0 * 512 + 1], lhsT=w1s[:, l, :], rhs=pT_[:, l:l + 1], start=(l == 0), stop=(l == 31))) for l in range(32)], [b_in], [PB[b0]])
            evac(hb[:], ps[:, b0 * 512:b0 * 512 + 1], [PB[b0]], [b_hb], eng="dve")
            S_.group("pe", [(lambda l=l: nc.tensor.matmul(ps[:, b1 * 512:b1 * 512 + NCMP], lhsT=w1s[:, l, :], rhs=kf[:, l:l + 16 * (NCMP - 1) + 1:16], start=(l == 0), stop=(l == 31))) for l in range(32)], [b_in], [PB[b1]])
            S_.op("act", lambda: nc.scalar.activation(out=gk[:, 0:NCMP], in_=ps[:, b1 * 512:b1 * 512 + NCMP], func=AF.Gelu_apprx_tanh, bias=hb[:, 0:1]), [PB[b1], b_hb], [b_gk])
            if kind == "k":
                S_.group("pe", [lambda: nc.tensor.matmul(ps[:, b2 * 512:b2 * 512 + NCMP], lhsT=w2s[:], rhs=gk[:, 0:NCMP], start=True, stop=True)], [b_in, b_gk], [PB[b2]])
                evac(kcT_sb[:, 0:NCMP], ps[:, b2 * 512:b2 * 512 + NCMP], [PB[b2]], [b_kc])
            else:
                for jc in range(NCC):
                    n = min(P, NCMP - jc * P)
                    S_.group("pe", [lambda: nc.tensor.matmul(ps[0:n, b2 * 512 + jc * P:b2 * 512 + (jc + 1) * P], lhsT=gk[:, jc * P:jc * P + n], rhs=w2s[:], start=True, stop=True)], [b_in, b_gk], [PB[b2]])
                    evac(vc_sb[0:n, jc, :], ps[0:n, b2 * 512 + jc * P:b2 * 512 + (jc + 1) * P], [PB[b2]], [b_vc])
        S_.barrier()

    if upto == 'A2':
        return dbg_return()

    TH = ceil_div(TOK, 512)
    HW_ = min(512, TOK)
    with ExitStack() as es:
        ps, PB = psum_banks(es)
        xo = sb(es, "xo", [P, KC, TOK], BF16); b_xo = Buf()
        xov = xTown.rearrange("(kc p) t -> p kc t", p=P)
        KG = min(8, KC)
        for k0 in range(0, KC, KG):
            for hf in range(2):
                S_.dma("pool", xo[:, k0:k0 + KG, hf * (TOK // 2):(hf + 1) * (TOK // 2)], xov[:, k0:k0 + KG, hf * (TOK // 2):(hf + 1) * (TOK // 2)], writes=[b_xo])
        wr = Ring(es, nc, "wga", [P, KC, 512], BF16, 2)
        st_u = Ring(es, nc, "stu", [P, 512], BF16, 3)
        st_v = Ring(es, nc, "stv2", [P, 512], F32, 3)
        wgv = w_ga.rearrange("(kc p) n -> p kc n", p=P)

        def load_w(cb):
            wt, b_w = wr.next()
            for k0 in range(0, KC, KG):
                S_.dma("pool", wt[:, k0:k0 + KG, :], wgv[:, k0:k0 + KG, cb * 512:(cb + 1) * 512], writes=[b_w])
            return wt, b_w

        bank = 0
        import os as _os
        _gm = _os.environ.get("GA_MODE", "")
        _cbs = {"u": [0, 1, 2, 3], "ucopy": [0, 1, 2, 3], "one": [0], "none": [], "v": [4, 5, 6, 7], "": list(range(8))}[_gm]
        _gfn = None if _gm == "ucopy" else AF.Gelu_apprx_tanh
        nxt = load_w(_cbs[0]) if _cbs else None
        for _ci, cb in enumerate(_cbs):
            wt, b_w = nxt
            if _ci + 1 < len(_cbs):
                nxt = load_w(_cbs[_ci + 1])
            if cb < 4:
                for cc in range(4):
                    for hf in range(TH):
                        bk = bank; bank = (bank + 1) % 8
                        S_.group("pe", [(lambda kc=kc: nc.tensor.matmul(ps[:, bk * 512:bk * 512 + HW_], lhsT=wt[:, kc, cc * P:(cc + 1) * P], rhs=xo[:, kc, hf * 512:hf * 512 + HW_], start=(kc == 0), stop=(kc == KC - 1))) for kc in range(KC)],
                                 [b_w, b_xo], [PB[bk]])
                        stg, b_s = st_u.next()
                        evac(stg[:, 0:HW_], ps[:, bk * 512:bk * 512 + HW_], [PB[bk]], [b_s], func=_gfn)
                        r0 = cb * 512 + cc * P
                        S_.dma("sp", guT_d[r0:r0 + P, hf * 512:hf * 512 + HW_], stg[:, 0:HW_], [b_s], [D_["guT"]])
            else:
                for tt in range(TOK // P):
                    bk = bank; bank = (bank + 1) % 8
                    S_.group("pe", [(lambda kc=kc: nc.tensor.matmul(ps[:, bk * 512:(bk + 1) * 512], lhsT=xo[:, kc, tt * P:(tt + 1) * P], rhs=wt[:, kc, :], start=(kc == 0), stop=(kc == KC - 1))) for kc in range(KC)],
                             [b_w, b_xo], [PB[bk]])
                    stg, b_s = st_v.next()
                    evac(stg[:], ps[:, bk * 512:(bk + 1) * 512], [PB[bk]], [b_s], func=AF.Gelu_apprx_tanh)
                    S_.dma("sp", gv_d[tt * P:(tt + 1) * P, (cb - 4) * 512:(cb - 3) * 512], stg[:], [b_s], [D_["gv"]])
        S_.barrier()

    if upto == 'GA':
        return dbg_return()

    with ExitStack() as es:
        ps, PB = psum_banks(es)
        gB = sb(es, "lng", [P, 2048], F32); bB = sb(es, "lnb", [P, 2048], F32); bsB = sb(es, "bsB", [P, 2048], F32)
        wmf = sb(es, "wmf", [P, 16 * P], F32); tri = sb(es, "tri", [P, P], F32); wm = sb(es, "wm", [P, 16, P], BF16)
        b_gb, b_wm = Buf(), Buf()
        S_.dma("sp", gB[:], ga_ln_g.to_broadcast([P, 2048]), writes=[b_gb])
        S_.dma("sp", bB[:], ga_ln_b.to_broadcast([P, 2048]), writes=[b_gb])
        S_.dma("sp", bsB[:], ga_bs.to_broadcast([P, 2048]), writes=[b_gb])
        S_.dma("sp", wmf[:], ga_wT, writes=[b_wm]); S_.dma("sp", tri[:], trilT, writes=[b_wm])
        for h in range(16):
            S_.op("dve", lambda: nc.vector.tensor_tensor(out=wm[:, h, :], in0=wmf[:, h * P:(h + 1) * P], in1=tri[:], op=ALU.mult), [b_wm], [b_wm])
        gvr = Ring(es, nc, "gvt", [P, 2048], F32, 2); junk = Ring(es, nc, "junk", [P, 2048], F32, 1)
        small = Ring(es, nc, "lnst", [P, 8], F32, 2)
        vln = Ring(es, nc, "vln", [P, 2048], BF16, 2); gur = Ring(es, nc, "gut", [P, 16, P], BF16, 2)
        tmpr = Ring(es, nc, "mixt", [P, 512], F32, 2); yar = Ring(es, nc, "yat", [P, 4, P], BF16, 3)
        guv = guT_d.rearrange("(h d) t -> d h t", d=P); yav = yaT_d.rearrange("(h d) t -> d h t", d=P)
        bank = 0
        for ci in range(TOK // P):
            gt, b_gt = gvr.next(); gu, b_gu = gur.next(); vl, b_vl = vln.next()
            S_.dma("sp", gt[:], gv_d[ci * P:(ci + 1) * P, :], [D_["gv"]], [b_gt])
            S_.dma("sp", gu[:], guv[:, :, ci * P:(ci + 1) * P], [D_["guT"]], [b_gu])
            layer_norm(junk, gt[:], b_gt, 2048, gB[:], bB[:], b_gb, vl[:], b_vl, small)
            for hb_ in range(4):
                bk = bank; bank = (bank + 1) % 8
                S_.group("pe", [(lambda j=j: nc.tensor.matmul(ps[:, bk * 512 + j * P:bk * 512 + (j + 1) * P], lhsT=vl[:, (4 * hb_ + j) * P:(4 * hb_ + j + 1) * P], rhs=wm[:, 4 * hb_ + j, :], start=True, stop=True)) for j in range(4)],
                         [b_vl, b_wm], [PB[bk]])
                tm, b_tm = tmpr.next(); ya, b_ya = yar.next()
                S_.op("dve", lambda: nc.vector.tensor_tensor(out=tm[:], in0=ps[:, bk * 512:(bk + 1) * 512], in1=bsB[:, hb_ * 512:(hb_ + 1) * 512], op=ALU.add), [PB[bk], b_gb], [b_tm])
                S_.op("dve", lambda: nc.vector.tensor_tensor(out=ya[:], in0=tm[:].rearrange("p (h i) -> p h i", i=P), in1=gu[:, 4 * hb_:4 * hb_ + 4, :], op=ALU.mult), [b_tm, b_gu], [b_ya])
                S_.dma("sp", yav[:, 4 * hb_:4 * hb_ + 4, ci * P:(ci + 1) * P], ya[:], [b_ya], [D_["yaT"]])
        S_.barrier()

    if upto == 'GB':
        return dbg_return()

    nsa = ExitStack()
    ybT = sb(nsa, "ybT", [P, 4, S], BF16); b_yb = Buf()
    extra["ybT"] = (ybT[:], [P, 4, S], BF16, b_yb)
    with ExitStack() as es:
        psf = es.enter_context(nc.psum_tensor("psf", [P, 6 * 512], F32))
        pst = es.enter_context(nc.psum_tensor("pst", [P, 2 * 1024], BF16))
        PBF = [Buf(True) for _ in range(6)]; PBT = [Buf(True) for _ in range(2)]
        qTa = sb(es, "qTa", [P, 4, S], BF16); ksT = sb(es, "ksT", [P, S], BF16); kwT = sb(es, "kwT", [P, S], BF16)
        vs = sb(es, "vs", [P, NQT, P], BF16); vw = sb(es, "vw", [P, NQT, P], BF16); gts = sb(es, "gts", [P, NQT, 12], F32)
        smk = sb(es, "smk", [P, NQT * NBLK], F32)
        b_q, b_ks, b_kw, b_vs, b_vw, b_g, b_smk = [Buf() for _ in range(7)]
        S_.dma("sp", qTa[:], qT_d.rearrange("(e d) s -> d e s", d=P), [D_["qT"]], [b_q])
        S_.dma("sp", ksT[:], kT_d["ks"], [D_["ks"]], [b_ks]); S_.dma("sp", kwT[:], kT_d["kw"], [D_["kw"]], [b_kw])
        S_.dma("sp", vs[:], vs_d.rearrange("(c p) d -> p c d", p=P), [D_["vs"]], [b_vs])
        S_.dma("sp", vw[:], vw_d.rearrange("(c p) d -> p c d", p=P), [D_["vw"]], [b_vw])
        S_.dma("sp", gts[:], gts_d.rearrange("(t p) k -> p t k", p=P), [D_["gts"]], [b_g])
        S_.dma("sp", smk[:], selmask, writes=[b_smk])
        ssb = Ring(es, nc, "ssb", [P, S], F32, 2); pbr = Ring(es, nc, "pb", [P, S], BF16, 2); pTr = Ring(es, nc, "pT", [P, NQT, P], BF16, 2)
        pn = sb(es, "pn", [P, 4, 260], F32); b_pn = Buf()
        S_.op("dve", lambda: nc.vector.memset(pn[:], 0.0), writes=[b_pn])
        sm_r = Ring(es, nc, "smr", [P, 8], F32, 6)
        pcb = Ring(es, nc, "pcb", [P, 256], BF16, 2); pef = Ring(es, nc, "pef", [P, 256], F32, 2)
        sel_t = Ring(es, nc, "selt", [P, 4, 260], F32, 1)
        m8r = Ring(es, nc, "m8", [P, 16], F32, 2); nmr = Ring(es, nc, "nm", [P, NBLK], F32, 2)
        oacc = Ring(es, nc, "oacc", [P, 512], F32, 2); oab = Ring(es, nc, "oab", [P, 512], BF16, 2)
        fb = {"s": 0, "o": 0, "t": 0}

        def sbank():
            k = fb["s"]; fb["s"] = (fb["s"] + 1) % 4
            return k

        def obank():
            k = 4 + fb["o"]; fb["o"] = (fb["o"] + 1) % 2
            return k

        def tbank():
            k = fb["t"]; fb["t"] = (fb["t"] + 1) % 2
            return k

        def softmax_tail(s_t, b_s, n, p_out, b_p, clamp):
            st, b_st = sm_r.next()
            S_.op("dve", lambda: nc.vector.memset(st[:], 0.0), writes=[b_st])
            S_.op("dve", lambda: nc.vector.reduce_max(out=st[:, 0:1], in_=s_t[:, 0:n], axis=AX.X), [b_s], [b_st])
            if clamp:
                S_.op("dve", lambda: nc.vector.tensor_scalar(out=st[:, 0:1], in0=st[:, 0:1], scalar1=-1000.0, scalar2=-1.0, op0=ALU.max, op1=ALU.mult), [b_st], [b_st])
            else:
                S_.op("dve", lambda: nc.vector.tensor_scalar(out=st[:, 0:1], in0=st[:, 0:1], scalar1=-1.0, scalar2=None, op0=ALU.mult), [b_st], [b_st])
            S_.op("act", lambda: nc.scalar.activation(out=p_out, in_=s_t[:, 0:n], func=AF.Exp, bias=st[:, 0:1], accum_out=st[:, 1:2]), [b_s, b_st], [b_p, b_st])
            S_.op("dve", lambda: nc.vector.tensor_scalar(out=st[:, 2:3], in0=st[:, 1:2], scalar1=1e-30, scalar2=None, op0=ALU.add), [b_st], [b_st])
            S_.op("dve", lambda: nc.vector.reciprocal(out=st[:, 2:3], in_=st[:, 2:3]), [b_st], [b_st])
            return st, b_st

        def transpose_chunks(p_t, b_p, widths, pT_t, b_pT):
            off = 0
            ch = 0
            while ch < len(widths):
                grp = widths[ch:ch + 8]
                tb_ = tbank()
                fns = []
                o2 = off
                for k, w in enumerate(grp):
                    fns.append(lambda k=k, w=w, o2=o2: nc.tensor.transpose(pst[0:w, tb_ * 1024 + k * P:tb_ * 1024 + (k + 1) * P], p_t[:, o2:o2 + w], ident_b[:]))
                    o2 += w
                S_.group("pe", fns, [b_p, b_ident], [PBT[tb_]])
                wmax = max(grp)
                evac(pT_t[0:wmax, ch:ch + len(grp), :], pst[0:wmax, tb_ * 1024:tb_ * 1024 + len(grp) * P].rearrange("p (c q) -> p c q", q=P), [PBT[tb_]], [b_pT])
                off = o2
                ch += len(grp)

        for t in range(NQT):
            oa, b_oa = oacc.next()
            qs = slice(t * P, (t + 1) * P)
            ncol = min(NCMP, 8 * t + 7)
            jb = max(0, 8 * t - 9)
            for e in range(4):
                bk = sbank()
                S_.group("pe", [lambda: nc.tensor.matmul(psf[:, bk * 512:bk * 512 + ncol], lhsT=qTa[:, e, qs], rhs=kcT_sb[:, 0:ncol], start=True, stop=True)], [b_q, b_kc], [PBF[bk]])
                sc, b_sc = pef.next()
                if jb > 0:
                    S_.op("dve", lambda: nc.vector.tensor_copy(out=sc[:, 0:jb], in_=psf[:, bk * 512:bk * 512 + jb]), [PBF[bk]], [b_sc])
                m0 = jb - (8 * t - 9)
                S_.op("dve", lambda: nc.vector.tensor_tensor(out=sc[:, jb:ncol], in0=psf[:, bk * 512 + jb:bk * 512 + ncol], in1=Bcmp[:, e, m0:m0 + ncol - jb], op=ALU.add), [PBF[bk], b_B], [b_sc])
                st, b_st = softmax_tail(sc, b_sc, ncol, sc[:, 0:ncol], b_sc, True)
                S_.op("dve", lambda: nc.vector.tensor_scalar(out=pn[:, e, 0:ncol], in0=sc[:, 0:ncol], scalar1=st[:, 2:3], scalar2=None, op0=ALU.mult), [b_sc, b_st], [b_pn])
                pc, b_pc = pcb.next()
                evac(pc[:, 0:ncol], pn[:, e, 0:ncol], [b_pn], [b_pc], eng="act")
                widths = [min(P, ncol - j * P) for j in range(ceil_div(ncol, P))]
                pT_t, b_pT = pTr.next()
                transpose_chunks(pc, b_pc, widths, pT_t, b_pT)
                ob = obank()
                S_.group("pe", [(lambda j=j, w=w: nc.tensor.matmul(psf[:, ob * 512:ob * 512 + P], lhsT=pT_t[0:w, j, :], rhs=vc_sb[0:w, j, :], start=(j == 0), stop=(j == len(widths) - 1))) for j, w in enumerate(widths)],
                         [b_pT, b_vc], [PBF[ob]])
                S_.op("dve", lambda: nc.vector.tensor_scalar(out=oa[:, e * P:(e + 1) * P], in0=psf[:, ob * 512:ob * 512 + P], scalar1=gts[:, t, 3 * e:3 * e + 1], scalar2=None, op0=ALU.mult), [PBF[ob], b_g], [b_oa])
            sl, b_sl = sel_t.next()
            S_.op("dve", lambda: nc.vector.tensor_tensor(out=sl[:, 0, :], in0=pn[:, 0, :], in1=pn[:, 1, :], op=ALU.add), [b_pn], [b_sl])
            S_.op("dve", lambda: nc.vector.tensor_tensor(out=sl[:, 0, :], in0=sl[:, 0, :], in1=pn[:, 2, :], op=ALU.add), [b_pn, b_sl], [b_sl])
            S_.op("dve", lambda: nc.vector.tensor_tensor(out=sl[:, 0, :], in0=sl[:, 0, :], in1=pn[:, 3, :], op=ALU.add), [b_pn, b_sl], [b_sl])
            S_.op("dve", lambda: nc.vector.tensor_reduce(out=sl[:, 1, 0:NBLK], in_=sl[:, 0, 0:4 * NBLK].rearrange("p (n r) -> p n r", r=4), axis=AX.X, op=ALU.add), [b_sl], [b_sl])
            S_.op("dve", lambda: nc.vector.tensor_reduce(out=sl[:, 2, 0:NBLK], in_=sl[:, 0, 1:4 * NBLK + 1].rearrange("p (n r) -> p n r", r=4), axis=AX.X, op=ALU.add), [b_sl], [b_sl])
            S_.op("dve", lambda: nc.vector.tensor_tensor(out=sl[:, 1, 0:NBLK], in0=sl[:, 1, 0:NBLK], in1=sl[:, 2, 0:NBLK], op=ALU.add), [b_sl], [b_sl])
            S_.op("dve", lambda: nc.vector.tensor_tensor(out=sl[:, 1, 0:NBLK], in0=sl[:, 1, 0:NBLK], in1=smk[:, t * NBLK:(t + 1) * NBLK], op=ALU.add), [b_sl, b_smk], [b_sl])
            m8, b_m8 = m8r.next(); nm, b_nm = nmr.next()
            S_.op("dve", lambda: nc.vector.max(out=m8[:, 0:8], in_=sl[:, 1, 0:NBLK]), [b_sl], [b_m8])
            S_.op("dve", lambda: nc.vector.match_replace(out=sl[:, 3, 0:NBLK], in_to_replace=m8[:, 0:8], in_values=sl[:, 1, 0:NBLK], imm_value=-1e30), [b_sl, b_m8], [b_sl])
            S_.op("dve", lambda: nc.vector.max(out=m8[:, 8:16], in_=sl[:, 3, 0:NBLK]), [b_sl], [b_m8])
            S_.op("dve", lambda: nc.vector.tensor_scalar(out=nm[:], in0=sl[:, 1, 0:NBLK], scalar1=m8[:, 15:16], scalar2=None, op0=ALU.is_ge), [b_sl, b_m8], [b_nm])
            S_.op("dve", lambda: nc.vector.tensor_scalar(out=nm[:], in0=nm[:], scalar1=-1.0, scalar2=-NEG, op0=ALU.add, op1=ALU.mult), [b_nm], [b_nm])
            kend = P * (t + 1)
            for e in range(4):
                s_t, b_s = ssb.next()
                for sg_ in range(ceil_div(kend, 512)):
                    w = min(512, kend - sg_ * 512)
                    bk = sbank()
                    S_.group("pe", [lambda: nc.tensor.matmul(psf[:, bk * 512:bk * 512 + w], lhsT=qTa[:, e, qs], rhs=ksT[:, sg_ * 512:sg_ * 512 + w], start=True, stop=True)], [b_q, b_ks], [PBF[bk]])
                    S_.op("dve", lambda: nc.vector.tensor_tensor(out=s_t[:, sg_ * 512:sg_ * 512 + w].rearrange("p (b k) -> p b k", k=64),
                                                                 in0=psf[:, bk * 512:bk * 512 + w].rearrange("p (b k) -> p b k", k=64),
                                                                 in1=nm[:, sg_ * 8:sg_ * 8 + w // 64].unsqueeze(2).to_broadcast([P, w // 64, 64]), op=ALU.add), [PBF[bk], b_nm], [b_s])
                n0 = max(0, kend - 256)
                S_.op("dve", lambda: nc.vector.tensor_tensor(out=s_t[:, n0:kend], in0=s_t[:, n0:kend], in1=Bwin[:, e, 640 - (kend - n0):640], op=ALU.add), [b_s, b_B], [b_s])
                p_t, b_p = pbr.next()
                st, b_st = softmax_tail(s_t, b_s, kend, p_t[:, 0:kend], b_p, False)
                S_.op("dve", lambda: nc.vector.tensor_tensor(out=st[:, 3:4], in0=st[:, 2:3], in1=gts[:, t, 3 * e + 1:3 * e + 2], op=ALU.mult), [b_st, b_g], [b_st])
                pT_t, b_pT = pTr.next()
                transpose_chunks(p_t, b_p, [P] * (t + 1), pT_t, b_pT)
                ob = obank()
                S_.group("pe", [(lambda j=j: nc.tensor.matmul(psf[:, ob * 512:ob * 512 + P], lhsT=pT_t[:, j, :], rhs=vs[:, j, :], start=(j == 0), stop=(j == t))) for j in range(t + 1)], [b_pT, b_vs], [PBF[ob]])
                S_.op("dve", lambda: nc.vector.scalar_tensor_tensor(out=oa[:, e * P:(e + 1) * P], in0=psf[:, ob * 512:ob * 512 + P], scalar=st[:, 3:4], in1=oa[:, e * P:(e + 1) * P], op0=ALU.mult, op1=ALU.add), [PBF[ob], b_st, b_oa], [b_oa])
            k0 = max(0, P * t - 512)
            wtot = kend - k0
            for e in range(4):
                s_t, b_s = ssb.next()
                for sg_ in range(ceil_div(wtot, 512)):
                    w = min(512, wtot - sg_ * 512)
                    bk = sbank()
                    S_.group("pe", [lambda: nc.tensor.matmul(psf[:, bk * 512:bk * 512 + w], lhsT=qTa[:, e, qs], rhs=kwT[:, k0 + sg_ * 512:k0 + sg_ * 512 + w], start=True, stop=True)], [b_q, b_kw], [PBF[bk]])
                    c0 = 640 - wtot + sg_ * 512
                    S_.op("dve", lambda: nc.vector.tensor_tensor(out=s_t[:, sg_ * 512:sg_ * 512 + w], in0=psf[:, bk * 512:bk * 512 + w], in1=Bwin[:, e, c0:c0 + w], op=ALU.add), [PBF[bk], b_B], [b_s])
                p_t, b_p = pbr.next()
                st, b_st = softmax_tail(s_t, b_s, wtot, p_t[:, 0:wtot], b_p, False)
                S_.op("dve", lambda: nc.vector.tensor_tensor(out=st[:, 3:4], in0=st[:, 2:3], in1=gts[:, t, 3 * e + 2:3 * e + 3], op=ALU.mult), [b_st, b_g], [b_st])
                nchw = wtot // P
                pT_t, b_pT = pTr.next()
                transpose_chunks(p_t, b_p, [P] * nchw, pT_t, b_pT)
                ob = obank()
                S_.group("pe", [(lambda j=j: nc.tensor.matmul(psf[:, ob * 512:ob * 512 + P], lhsT=pT_t[:, j, :], rhs=vw[:, k0 // P + j, :], start=(j == 0), stop=(j == nchw - 1))) for j in range(nchw)], [b_pT, b_vw], [PBF[ob]])
                S_.op("dve", lambda: nc.vector.scalar_tensor_tensor(out=oa[:, e * P:(e + 1) * P], in0=psf[:, ob * 512:ob * 512 + P], scalar=st[:, 3:4], in1=oa[:, e * P:(e + 1) * P], op0=ALU.mult, op1=ALU.add), [PBF[ob], b_st, b_oa], [b_oa])
            ob_, b_ob = oab.next()
            evac(ob_[:], oa[:], [b_oa], [b_ob], eng="act")
            tb_ = tbank()
            S_.group("pe", [(lambda e=e: nc.tensor.transpose(pst[:, tb_ * 1024 + e * P:tb_ * 1024 + (e + 1) * P], ob_[:, e * P:(e + 1) * P], ident_b[:])) for e in range(4)], [b_ob, b_ident], [PBT[tb_]])
            evac(ybT[:, :, qs], pst[:, tb_ * 1024:tb_ * 1024 + 512].rearrange("p (e q) -> p e q", q=P), [PBT[tb_]], [b_yb])
        S_.barrier()

    if upto == 'C':
        return dbg_return()

    with ExitStack() as es:
        ps, PB = psum_banks(es)
        wob = sb(es, "wob", [P, 4, D], BF16); b_wob = Buf()
        for hf in range(2):
            S_.dma("pool", wob[:, :, hf * (D // 2):(hf + 1) * (D // 2)], w_out_b.rearrange("(e p) n -> p e n", p=P)[:, :, hf * (D // 2):(hf + 1) * (D // 2)], writes=[b_wob])
        stg_r = Ring(es, nc, "d1st", [P, 512], F32, 4)
        bank = 0
        for tt in range(S // P):
            for cb in range(D // 512):
                bk = bank; bank = (bank + 1) % 8
                S_.group("pe", [(lambda e=e: nc.tensor.matmul(ps[:, bk * 512:(bk + 1) * 512], lhsT=ybT[:, e, tt * P:(tt + 1) * P], rhs=wob[:, e, cb * 512:(cb + 1) * 512], start=(e == 0), stop=(e == 3))) for e in range(4)],
                         [b_yb, b_wob], [PB[bk]])
                stg, b_s = stg_r.next()
                evac(stg[:], ps[:, bk * 512:(bk + 1) * 512], [PB[bk]], [b_s])
                for (j, l0, l1, g0, g1) in pieces_over(W1, cb * 512, (cb + 1) * 512):
                    S_.dma("sp", rs1_in[j][tt * P:(tt + 1) * P, l0:l1], stg[:, g0 - cb * 512:g1 - cb * 512], [b_s], [D_["rs1_in%d" % j]])
        for j in range(len(W1)):
            S_.cc("ReduceScatter", ALU.add, [[0, 1, 2, 3], [4, 5, 6, 7]], rs1_in[j], rs1_out[j], [D_["rs1_in%d" % j]], [D_["rs1_out%d" % j]])
        S_.barrier()
    nsa.close()

    if upto == 'D1':
        return dbg_return()

    with ExitStack() as es:
        ps, PB = psum_banks(es)
        yat = sb(es, "yaTs", [P, 16, TOK], BF16); b_ya = Buf()
        S_.dma("sp", yat[:], yaT_d.rearrange("(kc p) t -> p kc t", p=P), [D_["yaT"]], [b_ya])
        wr = Ring(es, nc, "woa", [P, 16, 512], BF16, 2)
        rin = Ring(es, nc, "d2r", [P, 512], F32, 3); xin = Ring(es, nc, "d2x", [P, 512], F32, 3)
        wov = w_out_a.rearrange("(kc p) n -> p kc n", p=P)

        def load_w(cb):
            wt, b_w = wr.next()
            S_.dma("pool", wt[:], wov[:, :, cb * 512:(cb + 1) * 512], writes=[b_w])
            return wt, b_w

        bank = 0
        nxt = load_w(0)
        for cb in range(D // 512):
            wt, b_w = nxt
            if cb + 1 < D // 512:
                nxt = load_w(cb + 1)
            for tt in range(TOK // P):
                bk = bank; bank = (bank + 1) % 8
                rt, b_r = rin.next(); xt_, b_x = xin.next()
                for (j, l0, l1, g0, g1) in pieces_over(W1, cb * 512, (cb + 1) * 512):
                    S_.dma("sp", rt[:, g0 - cb * 512:g1 - cb * 512], rs1_out[j][tt * P:(tt + 1) * P, l0:l1], [D_["rs1_out%d" % j]], [b_r])
                S_.dma("sp", xt_[:], xown[tt * P:(tt + 1) * P, cb * 512:(cb + 1) * 512], (), [b_x])
                S_.group("pe", [(lambda kc=kc: nc.tensor.matmul(ps[:, bk * 512:(bk + 1) * 512], lhsT=yat[:, kc, tt * P:(tt + 1) * P], rhs=wt[:, kc, :], start=(kc == 0), stop=(kc == 15))) for kc in range(16)],
                         [b_ya, b_w], [PB[bk]])
                S_.op("dve", lambda: nc.vector.tensor_tensor(out=rt[:], in0=ps[:, bk * 512:(bk + 1) * 512], in1=rt[:], op=ALU.add), [PB[bk], b_r], [b_r])
                S_.op("dve", lambda: nc.vector.scalar_tensor_tensor(out=rt[:], in0=xt_[:], scalar=ALPHA, in1=rt[:], op0=ALU.mult, op1=ALU.add), [b_x, b_r], [b_r])
                S_.dma("sp", z1_d[tt * P:(tt + 1) * P, cb * 512:(cb + 1) * 512], rt[:], [b_r], [D_["z1"]])
        S_.barrier()

    if upto == 'D2':
        return dbg_return()

    with ExitStack() as es:
        ps, PB = psum_banks(es)
        gB = sb(es, "l1g", [P, D], F32); bB = sb(es, "l1b", [P, D], F32); b_gb = Buf()
        S_.dma("sp", gB[:], ln1_g.to_broadcast([P, D]), writes=[b_gb]); S_.dma("sp", bB[:], ln1_b.to_broadcast([P, D]), writes=[b_gb])
        wrt = sb(es, "wrt", [P, KC, NE], F32); brt = sb(es, "brt", [1, NE], F32); b_wr = Buf()
        S_.dma("sp", wrt[:], w_router.rearrange("(kc p) n -> p kc n", p=P), writes=[b_wr]); S_.dma("sp", brt[:], b_router, writes=[b_wr])
        zr = Ring(es, nc, "z1t", [P, D], F32, 2); junk = Ring(es, nc, "junk1", [P, D], F32, 1); x1r = Ring(es, nc, "x1t", [P, D], F32, 2)
        small = Ring(es, nc, "l1st", [P, 8], F32, 2)
        xTf = Ring(es, nc, "xTf", [P, KC, P], F32, 1); xTb = Ring(es, nc, "xTbf", [P, KC, P], BF16, 2)
        lg = Ring(es, nc, "lg", [P, NE], F32, 2); ex = Ring(es, nc, "exg", [P, NE], F32, 2); m8r = Ring(es, nc, "rm8", [P, 16], F32, 2)
        gtr = Ring(es, nc, "gtr", [NE, P], F32, 2)
        bank = 0
        for tt in range(TOK // P):
            zt, b_z = zr.next(); x1, b_x1 = x1r.next()
            S_.dma("sp", zt[:], z1_d[tt * P:(tt + 1) * P, :], [D_["z1"]], [b_z])
            layer_norm(junk, zt[:], b_z, D, gB[:], bB[:], b_gb, x1[:], b_x1, small)
            S_.dma("sp", x1_d[tt * P:(tt + 1) * P, :], x1[:], [b_x1], [D_["x1"]])
            xf, b_xf = xTf.next(); xb, b_xb = xTb.next()
            for k0 in range(0, KC, 4):
                bk = bank; bank = (bank + 1) % 8
                n = min(4, KC - k0)
                S_.group("pe", [(lambda j=j: nc.tensor.transpose(ps[:, bk * 512 + j * P:bk * 512 + (j + 1) * P], x1[:, (k0 + j) * P:(k0 + j + 1) * P], ident_f[:])) for j in range(n)], [b_x1, b_ident], [PB[bk]])
                evac(xf[:, k0:k0 + n, :], ps[:, bk * 512:bk * 512 + n * P].rearrange("p (c q) -> p c q", q=P), [PB[bk]], [b_xf])
            evac(xb[:], xf[:], [b_xf], [b_xb], eng="act")
            for j, nk in enumerate(KA):
                S_.dma("sp", agx_in[j].rearrange("(kc p) t -> p kc t", p=P)[:, :, tt * P:(tt + 1) * P], xb[:, KA0[j]:KA0[j] + nk, :], [b_xb], [D_["agx_in%d" % j]])
            bk = bank; bank = (bank + 1) % 8
            fns = [(lambda kc=kc: nc.tensor.matmul(ps[:, bk * 512:bk * 512 + NE], lhsT=xf[:, kc, :], rhs=wrt[:, kc, :], start=(kc == 0), stop=False)) for kc in range(KC)]
            fns.append(lambda: nc.tensor.matmul(ps[:, bk * 512:bk * 512 + NE], lhsT=ones_f[:], rhs=brt[:], start=False, stop=True))
            S_.group("pe", fns, [b_xf, b_wr, b_ones], [PB[bk]])
            l_, b_l = lg.next(); e_, b_e = ex.next(); m8, b_m8 = m8r.next()
            evac(l_[:], ps[:, bk * 512:bk * 512 + NE], [PB[bk]], [b_l], eng="dve")
            S_.op("dve", lambda: nc.vector.memset(m8[:], 0.0), writes=[b_m8])
            S_.op("dve", lambda: nc.vector.max(out=m8[:, 0:8], in_=l_[:]), [b_l], [b_m8])
            S_.op("dve", lambda: nc.vector.tensor_scalar(out=m8[:, 8:9], in0=m8[:, 0:1], scalar1=-1.0, scalar2=None, op0=ALU.mult), [b_m8], [b_m8])
            S_.op("act", lambda: nc.scalar.activation(out=e_[:], in_=l_[:], func=AF.Exp, bias=m8[:, 8:9]), [b_l, b_m8], [b_e])
            S_.op("dve", lambda: nc.vector.tensor_scalar(out=l_[:], in0=l_[:], scalar1=m8[:, 3:4], scalar2=None, op0=ALU.is_ge), [b_l, b_m8], [b_l])
            S_.op("dve", lambda: nc.vector.tensor_tensor(out=e_[:], in0=e_[:], in1=l_[:], op=ALU.mult), [b_e, b_l], [b_e])
            S_.op("dve", lambda: nc.vector.reduce_sum(out=m8[:, 9:10], in_=e_[:], axis=AX.X), [b_e], [b_m8])
            S_.op("dve", lambda: nc.vector.reciprocal(out=m8[:, 10:11], in_=m8[:, 9:10]), [b_m8], [b_m8])
            S_.op("dve", lambda: nc.vector.tensor_scalar(out=e_[:], in0=e_[:], scalar1=m8[:, 10:11], scalar2=None, op0=ALU.mult), [b_e, b_m8], [b_e])
            bk = bank; bank = (bank + 1) % 8
            S_.group("pe", [lambda: nc.tensor.transpose(ps[0:NE, bk * 512:bk * 512 + P], e_[:], ident_f[:])], [b_e, b_ident], [PB[bk]])
            g_, b_g2 = gtr.next()
            evac(g_[:], ps[0:NE, bk * 512:bk * 512 + P], [PB[bk]], [b_g2], eng="dve")
            S_.dma("sp", agg_in[:, tt * P:(tt + 1) * P], g_[:], [b_g2], [D_["agg_in"]])
        for j in range(len(KA)):
            S_.cc("AllGather", ALU.bypass, [list(range(N_CORES))], agx_in[j], agx[j], [D_["agx_in%d" % j]], [D_["agx%d" % j]])
        S_.cc("AllGather", ALU.bypass, [list(range(N_CORES))], agg_in, agg, [D_["agg_in"]], [D_["agg"]])
        S_.barrier()

    if upto == 'D3':
        return dbg_return()

    with ExitStack() as es:
        ps, PB = psum_banks(es)
        PB2 = [Buf(True) for _ in range(4)]
        xblk = sb(es, "xblk", [P, KC, TOK], BF16); b_xb = Buf()
        gtb = Ring(es, nc, "gtb", [NE, TOK], F32, 2)
        selt = sb(es, "selt", [NE, EPC, P], F32); bgt = sb(es, "bgt", [P, EPC, 2, FC], F32); b_c = Buf()
        S_.dma("sp", selt[:], selB.rearrange("(i e) p -> e i p", e=NE), writes=[b_c])
        S_.dma("sp", bgt[:], bgu.rearrange("p (i t f) -> p i t f", t=2, f=FC), writes=[b_c])
        gBr = Ring(es, nc, "gBr", [P, TOK], F32, 2)
        wr = Ring(es, nc, "wgu", [P, KC, 256], BF16, 3)
        tg = Ring(es, nc, "tg", [P, TOK], F32, 2); tsg = Ring(es, nc, "tsg", [P, TOK], F32, 2); tl = Ring(es, nc, "tl", [P, TOK], F32, 2)
        ao = Ring(es, nc, "ao", [P, TOK], BF16, 3)
        slot = 0
        seq = [(r, i, fc) for r in range(N_CORES) for i in range(EPC) for fc in range(FC)]

        def load_w(r, i, fc):
            wt, b_w = wr.next()
            src = wgu[i * D:(i + 1) * D, :].rearrange("(kc p) n -> p kc n", p=P)[:, :, fc * 256:(fc + 1) * 256]
            h = KC // 2
            S_.dma("pool", wt[:, 0:h, :], src[:, 0:h, :], writes=[b_w])
            S_.dma("pool", wt[:, h:KC, :], src[:, h:KC, :], writes=[b_w])
            return wt, b_w

        pend = [load_w(*seq[0])]
        if len(seq) > 1:
            pend.append(load_w(*seq[1]))
        for n_, (r, i, fc) in enumerate(seq):
            if i == 0 and fc == 0:
                for j, nk in enumerate(KA):
                    S_.dma("sp", xblk[:, KA0[j]:KA0[j] + nk, :], agx[j][r * nk * P:(r + 1) * nk * P, :].rearrange("(kc p) t -> p kc t", p=P), [D_["agx%d" % j]], [b_xb])
                gt_, b_gt = gtb.next()
                S_.dma("sp", gt_[:], agg[r * NE:(r + 1) * NE, :], [D_["agg"]], [b_gt])
            if fc == 0:
                gb_, b_gb_ = gBr.next()
                sl_ = slot; slot = (slot + 1) % 4
                S_.group("pe", [(lambda hf=hf: nc.tensor.matmul(ps[:, sl_ * 1024 + hf * 512:sl_ * 1024 + hf * 512 + HW_], lhsT=selt[:, i, :], rhs=gt_[:, hf * 512:hf * 512 + HW_], start=True, stop=True)) for hf in range(TH)],
                         [b_c, b_gt], [PB2[sl_]])
                evac(gb_[:], ps[:, sl_ * 1024:sl_ * 1024 + TOK], [PB2[sl_]], [b_gb_], eng="act")
            wt, b_w = pend.pop(0)
            if n_ + 2 < len(seq):
                pend.append(load_w(*seq[n_ + 2]))
            sg_ = slot; slot = (slot + 1) % 4
            sl2 = slot; slot = (slot + 1) % 4
            fns = []
            for kc in range(KC):
                for ty, s_ in ((0, sg_), (1, sl2)):
                    for hf in range(TH):
                        fns.append(lambda kc=kc, ty=ty, s_=s_, hf=hf: nc.tensor.matmul(ps[:, s_ * 1024 + hf * 512:s_ * 1024 + hf * 512 + HW_], lhsT=wt[:, kc, ty:256:2], rhs=xblk[:, kc, hf * 512:hf * 512 + HW_], start=(kc == 0), stop=(kc == KC - 1)))
            S_.group("pe", fns, [b_w, b_xb], [PB2[sg_], PB2[sl2]])
            g_, b_g_ = tg.next(); s2, b_s2 = tsg.next(); l_, b_l_ = tl.next(); a_, b_a = ao.next()
            pg = ps[:, sg_ * 1024:sg_ * 1024 + TOK]; pl = ps[:, sl2 * 1024:sl2 * 1024 + TOK]
            S_.op("dve", lambda: nc.vector.tensor_scalar(out=g_[:], in0=pg, scalar1=bgt[:, i, 0, fc:fc + 1], scalar2=7.0, op0=ALU.add, op1=ALU.min), [PB2[sg_], b_c], [b_g_])
            S_.op("act", lambda: nc.scalar.activation(out=s2[:], in_=g_[:], func=AF.Sigmoid, scale=1.702), [b_g_], [b_s2])
            S_.op("dve", lambda: nc.vector.tensor_scalar(out=l_[:], in0=pl, scalar1=bgt[:, i, 1, fc:fc + 1], scalar2=7.0, op0=ALU.add, op1=ALU.min), [PB2[sl2], b_c], [b_l_])
            S_.op("dve", lambda: nc.vector.tensor_scalar(out=l_[:], in0=l_[:], scalar1=-7.0, scalar2=1.0, op0=ALU.max, op1=ALU.add), [b_l_], [b_l_])
            S_.op("dve", lambda: nc.vector.tensor_tensor(out=g_[:], in0=g_[:], in1=s2[:], op=ALU.mult), [b_g_, b_s2], [b_g_])
            S_.op("dve", lambda: nc.vector.tensor_tensor(out=l_[:], in0=l_[:], in1=gb_[:], op=ALU.mult), [b_l_, b_gb_], [b_l_])
            S_.op("dve", lambda: nc.vector.tensor_tensor(out=a_[:], in0=g_[:], in1=l_[:], op=ALU.mult), [b_g_, b_l_], [b_a])
            r0 = i * DFF + fc * P
            S_.dma("sp", act_d[r0:r0 + P, r * TOK:(r + 1) * TOK], a_[:], [b_a], [D_["act"]])
        S_.barrier()

    if upto == 'E':
        return dbg_return()

    NTT = TOK // P
    with ExitStack() as es:
        ps, PB = psum_banks(es)
        bds = sb(es, "bds", [NE, D], F32); b_bd = Buf()
        S_.dma("sp", bds[:], bdsel, writes=[b_bd])
        gtb = Ring(es, nc, "gtb2", [NE, TOK], F32, 2)
        ar = Ring(es, nc, "actb", [P, FC, TOK], BF16, 2)
        wr = Ring(es, nc, "wdb", [P, FC, 512], BF16, 2)
        stg_r = Ring(es, nc, "fst", [P, 512], F32, 4)
        for r in range(N_CORES):
            gt_, b_gt = gtb.next()
            S_.dma("sp", gt_[:], agg[r * NE:(r + 1) * NE, :], [D_["agg"]], [b_gt])
            for db in range(D // 512):
                S_.group("pe", [(lambda tt=tt: nc.tensor.matmul(ps[:, tt * 512:(tt + 1) * 512], lhsT=gt_[:, tt * P:(tt + 1) * P], rhs=bds[:, db * 512:(db + 1) * 512], start=True, stop=False)) for tt in range(NTT)],
                         [b_gt, b_bd], PB[0:NTT])
                for i in range(EPC):
                    at, b_at = ar.next(); wt, b_w = wr.next()
                    S_.dma("sp", at[:], act_d[i * DFF:(i + 1) * DFF, r * TOK:(r + 1) * TOK].rearrange("(fc p) t -> p fc t", p=P), [D_["act"]], [b_at])
                    S_.dma("pool", wt[:], wd[i * DFF:(i + 1) * DFF, db * 512:(db + 1) * 512].rearrange("(fc p) n -> p fc n", p=P), writes=[b_w])
                    fns = []
                    for fc in range(FC):
                        for tt in range(NTT):
                            fns.append(lambda fc=fc, tt=tt: nc.tensor.matmul(ps[:, tt * 512:(tt + 1) * 512], lhsT=at[:, fc, tt * P:(tt + 1) * P], rhs=wt[:, fc, :], start=False, stop=(i == EPC - 1 and fc == FC - 1)))
                    S_.group("pe", fns, [b_at, b_w], PB[0:NTT])
                for tt in range(NTT):
                    stg, b_s = stg_r.next()
                    evac(stg[:], ps[:, tt * 512:(tt + 1) * 512], [PB[tt]], [b_s])
                    for (j, l0, l1, g0, g1) in pieces_over(W2, db * 512, (db + 1) * 512):
                        S_.dma("sp", part_d[j][r * TOK + tt * P:r * TOK + (tt + 1) * P, l0:l1], stg[:, g0 - db * 512:g1 - db * 512], [b_s], [D_["part%d" % j]])
        for j in range(len(W2)):
            S_.cc("ReduceScatter", ALU.add, [list(range(N_CORES))], part_d[j], ffn_d[j], [D_["part%d" % j]], [D_["ffn%d" % j]])
        S_.barrier()

    if upto == 'F':
        return dbg_return()

    with ExitStack() as es:
        gB = sb(es, "l2g", [P, D], F32); bB = sb(es, "l2b", [P, D], F32); b_gb = Buf()
        S_.dma("sp", gB[:], ln2_g.to_broadcast([P, D]), writes=[b_gb]); S_.dma("sp", bB[:], ln2_b.to_broadcast([P, D]), writes=[b_gb])
        fr = Ring(es, nc, "ffnt", [P, D], F32, 2); xr_ = Ring(es, nc, "x1t2", [P, D], F32, 2); junk = Ring(es, nc, "junk2", [P, D], F32, 1)
        orr = Ring(es, nc, "outt", [P, D], F32, 2); small = Ring(es, nc, "l2st", [P, 8], F32, 2)
        b_out = Buf()
        for tt in range(TOK // P):
            ft, b_f = fr.next(); xt_, b_x = xr_.next(); ot, b_o = orr.next()
            off_ = 0
            for j, w in enumerate(W2):
                S_.dma("sp", ft[:, off_:off_ + w], ffn_d[j][tt * P:(tt + 1) * P, :], [D_["ffn%d" % j]], [b_f])
                off_ += w
            S_.dma("sp", xt_[:], x1_d[tt * P:(tt + 1) * P, :], [D_["x1"]], [b_x])
            S_.op("dve", lambda: nc.vector.scalar_tensor_tensor(out=ft[:], in0=xt_[:], scalar=ALPHA, in1=ft[:], op0=ALU.mult, op1=ALU.add), [b_x, b_f], [b_f])
            layer_norm(junk, ft[:], b_f, D, gB[:], bB[:], b_gb, ot[:], b_o, small)
            S_.dma("sp", out[tt * P:(tt + 1) * P, :], ot[:], [b_o], [b_out])
        S_.barrier()
    top.close()
    return nc


_CACHE = {}


def run(cfg, inputs):
    maps = host_inputs(cfg, inputs)
    key = (cfg.D, cfg.S, cfg.NE, cfg.DFF)
    if key not in _CACHE:
        _CACHE[key] = build(cfg)
    res = run_bass_kernel_spmd(_CACHE[key], maps, core_ids=list(range(N_CORES)))
    outs = [np.asarray(r["out"]) for r in res.results]
    return np.concatenate(outs, axis=0).reshape(cfg.B, cfg.S, cfg.D).astype(np.float32)


def kernel(**inputs):
    return run(FULL, inputs)
```
